# Optimizing a Trainium2 kernel written in Bass

```python
import math
import jax, jax.numpy as jnp
from jax import lax
import numpy as np

D_MODEL = 4096
BATCH = 1
SEQ = 8192
DEPTH = 4

HEAD_DIM = 128
N_A_LAYERS = max(1, DEPTH // 2)
N_B_LAYERS = DEPTH - N_A_LAYERS
A_GROUPS = ((128, 1), (512, 4), (2048, 16))
A_HEADS = D_MODEL // 256
A_WIDTH = A_HEADS * HEAD_DIM
A_BLOCK = 128
B_HEADS = D_MODEL // HEAD_DIM
B_KV_HEADS = B_HEADS // 4
B_REP = B_HEADS // B_KV_HEADS
MOBA_BLOCK = 256
MOBA_TOPK = 3
MOBA_Q_CHUNK = 16
MOE_GROUPS = 4
MOE_EXPERTS_PER_GROUP = 8
N_EXPERTS = MOE_GROUPS * MOE_EXPERTS_PER_GROUP
D_EXPERT = D_MODEL // 16
MOE_TOPK = 2
MOE_BLOCK = 128
ADA_RANK = D_MODEL // 16
ALPHA = (2 * DEPTH) ** 0.25
BETA = (8 * DEPTH) ** -0.25
LN_EPS = 1e-5
NEG = -1e30

kernel_name = "yoco_dilated_moba_hmoe_trunk"


def layer_norm(x, gain, bias):
    xf = x.astype(jnp.float32)
    mu = jnp.mean(xf, axis=-1, keepdims=True)
    var = jnp.mean(jnp.square(xf - mu), axis=-1, keepdims=True)
    return ((xf - mu) * lax.rsqrt(var + LN_EPS)).astype(x.dtype) * gain + bias


def alibi_slopes(n):
    return jnp.exp2(-8.0 * jnp.arange(1, n + 1, dtype=jnp.float32) / n)


def modulation(c, w_down, w_up, b_up, n_parts):
    m = (jax.nn.silu(c) @ w_down) @ w_up + b_up
    return [t[:, None, :] for t in jnp.split(m, n_parts, axis=-1)]


def dilated_branch(q, k, v, n_steps, dilation, slopes):
    b, s, h, e = q.shape
    n = s // dilation
    nb = -(-n // A_BLOCK)
    n_pad = nb * A_BLOCK

    def strided(t):
        t = t.reshape(b, n, dilation, h, e).transpose(0, 2, 1, 3, 4)
        t = jnp.pad(t, ((0, 0), (0, 0), (0, n_pad - n), (0, 0), (0, 0)))
        return t.reshape(b, dilation, nb, A_BLOCK, h, e)

    def band(t):
        prev = jnp.pad(t, ((0, 0), (0, 0), (1, 0), (0, 0), (0, 0), (0, 0)))[:, :, :nb]
        return jnp.concatenate([prev, t], axis=3)

    qs = strided(q)
    kb = band(strided(k))
    vb = band(strided(v))
    sc = jnp.einsum('brnqhe,brnkhe->brnhqk', qs, kb,
                    preferred_element_type=jnp.float32) / math.sqrt(e)
    kj = jnp.arange(2 * A_BLOCK)
    step = jnp.arange(A_BLOCK)[:, None] + A_BLOCK - kj[None, :]
    in_band = (step >= 0) & (step <= n_steps)
    exists = (jnp.arange(nb)[:, None, None] > 0) | (kj[None, None, :] >= A_BLOCK)
    valid = in_band[None] & exists
    bias = -slopes[:, None, None] * (step * dilation).astype(jnp.float32)[None]
    sc = jnp.where(valid[:, None], sc + bias, NEG)
    lse = jax.nn.logsumexp(sc, axis=-1)
    p = jnp.exp(sc - lse[..., None]).astype(v.dtype)
    o = jnp.einsum('brnhqk,brnkhe->brnqhe', p, vb)
    o = o.reshape(b, dilation, n_pad, h, e)[:, :, :n].transpose(0, 2, 1, 3, 4).reshape(b, s, h, e)
    lse = lse.transpose(0, 1, 2, 4, 3).reshape(b, dilation, n_pad, h)[:, :, :n]
    lse = lse.transpose(0, 2, 1, 3).reshape(b, s, h)
    return o, lse


def dilated_mixture_attention(h, w_qkv, w_o):
    b, s, _ = h.shape
    qkv = (h @ w_qkv).reshape(b, s, len(A_GROUPS), 3, A_HEADS, HEAD_DIM)
    slopes = alibi_slopes(A_HEADS)
    outs, lses = [], []
    for g, (window, dilation) in enumerate(A_GROUPS):
        o, lse = dilated_branch(qkv[:, :, g, 0], qkv[:, :, g, 1], qkv[:, :, g, 2],
                                window // dilation, dilation, slopes)
        outs.append(o)
        lses.append(lse)
    wts = jax.nn.softmax(jnp.stack(lses), axis=0)
    o = jnp.sum(jnp.stack(outs).astype(jnp.float32) * wts[..., None], axis=0).astype(h.dtype)
    return o.reshape(b, s, A_WIDTH) @ w_o


def shared_kv(x, c, kv_ada_down, kv_ada_up, kv_ada_bias, kv_w):
    shift, scale = modulation(c, kv_ada_down, kv_ada_up, kv_ada_bias, 2)
    hk = x * (1 + scale) + shift
    b, s, _ = x.shape
    kv = (hk @ kv_w).reshape(b, s, 2, B_KV_HEADS, HEAD_DIM)
    n_blk = -(-s // MOBA_BLOCK)
    pad = ((0, 0), (0, n_blk * MOBA_BLOCK - s), (0, 0), (0, 0))
    k = jnp.pad(kv[:, :, 0], pad)
    v = jnp.pad(kv[:, :, 1], pad)
    k_mean = k.astype(jnp.float32).reshape(b, n_blk, MOBA_BLOCK, B_KV_HEADS, HEAD_DIM).mean(axis=2)
    return k, v, k_mean


def moba_attention(h, w_q, w_o, k, v, k_mean):
    b, s, _ = h.shape
    n_blk = k.shape[1] // MOBA_BLOCK
    topk = min(MOBA_TOPK, n_blk)
    scale = 1.0 / math.sqrt(HEAD_DIM)
    q = (h @ w_q).reshape(b, s, B_KV_HEADS, B_REP, HEAD_DIM)
    slopes = alibi_slopes(B_HEADS).reshape(B_KV_HEADS, B_REP)
    pos = jnp.arange(s)
    gate = jnp.einsum('bsgre,bnge->bsgrn', q.astype(jnp.float32), k_mean)
    past = jnp.arange(n_blk)[None, :] < (pos // MOBA_BLOCK)[:, None]
    gate = jnp.where(past[None, :, None, None, :], gate, -jnp.inf)
    top_val, top_idx = lax.top_k(gate, topk)
    sel_ok = jnp.isfinite(top_val)
    kb = k.reshape(b, n_blk, MOBA_BLOCK, B_KV_HEADS, HEAD_DIM).transpose(0, 3, 1, 2, 4)
    vb = v.reshape(b, n_blk, MOBA_BLOCK, B_KV_HEADS, HEAD_DIM).transpose(0, 3, 1, 2, 4)
    bi = jnp.arange(b)[:, None, None, None, None]
    gi = jnp.arange(B_KV_HEADS)[None, None, :, None, None]
    offs = jnp.arange(MOBA_BLOCK)
    n_sel = topk * MOBA_BLOCK

    def chunk_fn(args):
        qc, idx, ok, t0 = args
        t = t0 + jnp.arange(MOBA_Q_CHUNK)
        k_sel = kb[bi, gi, idx]
        v_sel = vb[bi, gi, idx]
        s_sel = jnp.einsum('bcgre,bcgrike->bcgrik', qc, k_sel,
                           preferred_element_type=jnp.float32) * scale
        dist_sel = (t[None, :, None, None, None, None]
                    - (idx[..., None] * MOBA_BLOCK + offs)).astype(jnp.float32)
        s_sel = jnp.where(ok[..., None],
                          s_sel - slopes[None, None, :, :, None, None] * dist_sel, NEG)
        start = (t0 // MOBA_BLOCK) * MOBA_BLOCK
        k_own = lax.dynamic_slice_in_dim(k, start, MOBA_BLOCK, axis=1)
        v_own = lax.dynamic_slice_in_dim(v, start, MOBA_BLOCK, axis=1)
        s_own = jnp.einsum('bcgre,bkge->bcgrk', qc, k_own,
                           preferred_element_type=jnp.float32) * scale
        dist_own = t[:, None] - (start + offs)[None, :]
        s_own = jnp.where((dist_own >= 0)[None, :, None, None, :],
                          s_own - slopes[None, None, :, :, None]
                          * dist_own.astype(jnp.float32)[None, :, None, None, :], NEG)
        probs = jax.nn.softmax(
            jnp.concatenate([s_sel.reshape(s_sel.shape[:4] + (n_sel,)), s_own], axis=-1),
            axis=-1).astype(v.dtype)
        p_sel = probs[..., :n_sel].reshape(s_sel.shape)
        p_own = probs[..., n_sel:]
        return (jnp.einsum('bcgrik,bcgrike->bcgre', p_sel, v_sel)
                + jnp.einsum('bcgrk,bkge->bcgre', p_own, v_own))

    n_chunks = s // MOBA_Q_CHUNK

    def to_chunks(t):
        return t.reshape((b, n_chunks, MOBA_Q_CHUNK) + t.shape[2:]).swapaxes(0, 1)

    t0s = jnp.arange(n_chunks) * MOBA_Q_CHUNK
    o = lax.map(chunk_fn, (to_chunks(q), to_chunks(top_idx), to_chunks(sel_ok), t0s))
    o = o.swapaxes(0, 1).reshape(b, s, B_HEADS * HEAD_DIM)
    return o @ w_o


def hierarchical_moe(h, w_group, b_group, w_expert, b_expert, w_gate, w_up, w_down):
    b, s, d = h.shape
    n = b * s
    xt = h.reshape(n, d)
    g_logits = (xt @ w_group + b_group).astype(jnp.float32)
    g_sel = jnp.argmax(g_logits, axis=-1)
    p_group = jnp.take_along_axis(jax.nn.softmax(g_logits, axis=-1), g_sel[:, None], axis=1)
    e_logits = (jnp.einsum('nd,gde->nge', xt, w_expert) + b_expert).astype(jnp.float32)
    e_logits = jnp.take_along_axis(e_logits, g_sel[:, None, None], axis=1)[:, 0]
    top_val, top_idx = lax.top_k(e_logits, MOE_TOPK)
    wts = jax.nn.softmax(top_val, axis=-1) * p_group
    expert_id = (g_sel[:, None] * MOE_EXPERTS_PER_GROUP + top_idx).astype(jnp.int32)
    a = n * MOE_TOPK
    e_flat = expert_id.reshape(a)
    tok_flat = jnp.repeat(jnp.arange(n, dtype=jnp.int32), MOE_TOPK)
    order = jnp.argsort(e_flat)
    e_sorted = e_flat[order]
    tok_sorted = tok_flat[order]
    w_sorted = wts.reshape(a)[order]
    counts = jnp.zeros((N_EXPERTS,), jnp.int32).at[e_flat].add(1)
    starts = jnp.cumsum(counts) - counts
    padded = (counts + MOE_BLOCK - 1) // MOE_BLOCK * MOE_BLOCK
    ends_p = jnp.cumsum(padded)
    dest = (ends_p - padded)[e_sorted] + jnp.arange(a, dtype=jnp.int32) - starts[e_sorted]
    n_blocks = -(-a // MOE_BLOCK) + N_EXPERTS
    slot_tok = jnp.full((n_blocks * MOE_BLOCK,), n, jnp.int32).at[dest].set(tok_sorted)
    rows = jnp.concatenate([xt, jnp.zeros((1, d), xt.dtype)], axis=0)[slot_tok]
    rows = rows.reshape(n_blocks, MOE_BLOCK, d)
    block_start = jnp.arange(n_blocks, dtype=jnp.int32) * MOE_BLOCK
    block_expert = jnp.minimum(jnp.searchsorted(ends_p, block_start, side='right'), N_EXPERTS - 1)

    def expert_block(args):
        xb, e = args
        return (jax.nn.silu(xb @ w_gate[e]) * (xb @ w_up[e])) @ w_down[e]

    out = lax.map(expert_block, (rows, block_expert)).reshape(-1, d)
    y = jax.ops.segment_sum(out[dest] * w_sorted[:, None].astype(out.dtype), tok_sorted,
                            num_segments=n)
    return y.reshape(b, s, d)


def setup_inputs(seed: int = 0) -> dict:
    key = jax.random.key(seed)
    ks = jax.random.split(key, 24)
    D = D_MODEL
    f32 = jnp.float32

    def nrm(k, shape, scale):
        return jax.random.normal(k, shape, f32) * scale

    qkv_col_scale = jnp.ones((len(A_GROUPS), 3, A_WIDTH), f32).at[:, 2].set(BETA).reshape(-1)
    kv_width = B_KV_HEADS * HEAD_DIM
    kv_col_scale = jnp.concatenate([jnp.ones((kv_width,), f32), jnp.full((kv_width,), BETA, f32)])
    return {
        'x': nrm(ks[0], (BATCH, SEQ, D), 1.0),
        'c': nrm(ks[1], (BATCH, D), 1.0),
        'ada_down': nrm(ks[2], (DEPTH, D, ADA_RANK), D ** -0.5),
        'ada_up': nrm(ks[3], (DEPTH, ADA_RANK, 6 * D), 0.1 * ADA_RANK ** -0.5),
        'ada_bias': nrm(ks[4], (DEPTH, 6 * D), 0.01),
        'ln_gain': 1.0 + nrm(ks[5], (DEPTH, 2, D), 0.02),
        'ln_bias': nrm(ks[6], (DEPTH, 2, D), 0.02),
        'a_w_qkv': nrm(ks[7], (N_A_LAYERS, D, 9 * A_WIDTH), D ** -0.5) * qkv_col_scale,
        'a_w_o': nrm(ks[8], (N_A_LAYERS, A_WIDTH, D), BETA * A_WIDTH ** -0.5),
        'kv_ada_down': nrm(ks[9], (D, ADA_RANK), D ** -0.5),
        'kv_ada_up': nrm(ks[10], (ADA_RANK, 2 * D), 0.1 * ADA_RANK ** -0.5),
        'kv_ada_bias': nrm(ks[11], (2 * D,), 0.01),
        'kv_w': nrm(ks[12], (D, 2 * kv_width), D ** -0.5) * kv_col_scale,
        'b_w_q': nrm(ks[13], (N_B_LAYERS, D, B_HEADS * HEAD_DIM), D ** -0.5),
        'b_w_o': nrm(ks[14], (N_B_LAYERS, B_HEADS * HEAD_DIM, D), BETA * (B_HEADS * HEAD_DIM) ** -0.5),
        'moe_w_group': nrm(ks[15], (DEPTH, D, MOE_GROUPS), D ** -0.5),
        'moe_b_group': nrm(ks[16], (DEPTH, MOE_GROUPS), 0.01),
        'moe_w_expert': nrm(ks[17], (DEPTH, MOE_GROUPS, D, MOE_EXPERTS_PER_GROUP), D ** -0.5),
        'moe_b_expert': nrm(ks[18], (DEPTH, MOE_GROUPS, MOE_EXPERTS_PER_GROUP), 0.01),
        'moe_w_gate': nrm(ks[19], (DEPTH, N_EXPERTS, D, D_EXPERT), D ** -0.5),
        'moe_w_up': nrm(ks[20], (DEPTH, N_EXPERTS, D, D_EXPERT), D ** -0.5),
        'moe_w_down': nrm(ks[21], (DEPTH, N_EXPERTS, D_EXPERT, D), BETA * D_EXPERT ** -0.5),
    }


def reference(x, c, ada_down, ada_up, ada_bias, ln_gain, ln_bias, a_w_qkv, a_w_o,
              kv_ada_down, kv_ada_up, kv_ada_bias, kv_w, b_w_q, b_w_o,
              moe_w_group, moe_b_group, moe_w_expert, moe_b_expert,
              moe_w_gate, moe_w_up, moe_w_down):
    for l in range(DEPTH):
        shift1, scale1, gate1, shift2, scale2, gate2 = modulation(
            c, ada_down[l], ada_up[l], ada_bias[l], 6)
        hm = x * (1 + scale1) + shift1
        if l < N_A_LAYERS:
            mix = dilated_mixture_attention(hm, a_w_qkv[l], a_w_o[l])
        else:
            if l == N_A_LAYERS:
                k_sh, v_sh, k_mean = shared_kv(x, c, kv_ada_down, kv_ada_up, kv_ada_bias, kv_w)
            j = l - N_A_LAYERS
            mix = moba_attention(hm, b_w_q[j], b_w_o[j], k_sh, v_sh, k_mean)
        x = layer_norm(ALPHA * x + (1 + gate1) * mix, ln_gain[l, 0], ln_bias[l, 0])
        hf = x * (1 + scale2) + shift2
        ffn = hierarchical_moe(hf, moe_w_group[l], moe_b_group[l], moe_w_expert[l],
                               moe_b_expert[l], moe_w_gate[l], moe_w_up[l], moe_w_down[l])
        x = layer_norm(ALPHA * x + (1 + gate2) * ffn, ln_gain[l, 1], ln_bias[l, 1])
    return x
```

```python
import math
import contextlib
import numpy as np
import ml_dtypes
import concourse.bass as bass
import concourse.mybir as mybir
from concourse.bass_utils import run_bass_kernel_spmd

F32 = mybir.dt.float32
BF16 = mybir.dt.bfloat16
AF = mybir.ActivationFunctionType
ALU = mybir.AluOpType
AX = mybir.AxisListType
NPBF = ml_dtypes.bfloat16

D = 4096
S = 8192
TL = 1024
NCORE = 8
ALPHA = 8.0 ** 0.25
LN_EPS = 1e-5
SCALE = 1.0 / math.sqrt(128.0)
A_GROUPS = ((128, 1), (512, 4), (2048, 16))
NREL = (2, 5, 17)
GOFF = (0, 2, 7)
RING = 24
ENGS = ("pe", "act", "dve", "pool", "sp")
RG = [list(range(NCORE))]
EPOCH = 16000


class Prog:
    def __init__(self, nc, n_dma_sems=10):
        self.nc = nc
        self.ops = {e: [] for e in ENGS}
        self.cnt = {e: 0 for e in ENGS}
        self.cnt["cc"] = 0
        self.res = {}
        self.waited = {e: {} for e in ENGS}
        self.n_dma_sems = n_dma_sems
        self.dma_cnt = {}
        self.dma_rr = {e: 0 for e in ENGS}

    def _deps(self, eng, reads, writes, extra=()):
        deps = []
        for k in reads:
            r = self.res.get(k)
            if r is not None and r[0] is not None:
                deps.append(r[0])
        for k in writes:
            r = self.res.get(k)
            if r is not None:
                if r[0] is not None:
                    deps.append(r[0])
                deps.extend(r[1])
        deps.extend(extra)
        m = {}
        for (sk, val) in deps:
            if sk[0] == "pe" and eng == "pe":
                continue
            if self.waited[eng].get(sk, 0) >= val:
                continue
            m[sk] = max(m.get(sk, 0), val)
        for sk, val in m.items():
            self.waited[eng][sk] = val
        return list(m.items())

    def _commit(self, token, reads, writes):
        for k in writes:
            self.res[k] = [token, []]
        for k in reads:
            if k in writes:
                continue
            r = self.res.setdefault(k, [None, []])
            r[1].append(token)
            if len(r[1]) > 64:
                mm = {}
                for (sk, v) in r[1]:
                    mm[sk] = max(mm.get(sk, 0), v)
                r[1] = list(mm.items())

    def op(self, eng, name, kw, reads=(), writes=()):
        reads, writes = tuple(reads), tuple(writes)
        waits = self._deps(eng, reads, writes)
        i = self.cnt[eng]
        self.cnt[eng] += 1
        sk = (eng, i // EPOCH)
        token = (sk, i % EPOCH + 1)
        self.ops[eng].append(("c", name, kw, waits, sk))
        self._commit(token, reads, writes)
        return token

    def dma(self, q, out, in_, reads=(), writes=()):
        reads, writes = tuple(reads), tuple(writes)
        i = self.dma_rr[q]
        self.dma_rr[q] = (i + 1) % self.n_dma_sems
        sk = ("dma", q, i)
        prev = self.dma_cnt.get(sk, 0)
        ex = [(sk, 16 * prev)] if prev > 0 else []
        waits = self._deps(q, reads, writes, ex)
        self.dma_cnt[sk] = prev + 1
        token = (sk, 16 * (prev + 1))
        self.ops[q].append(("d", "dma_start", dict(out=out, in_=in_), waits, sk))
        self._commit(token, reads, writes)
        return token

    def coll(self, kind, alu, in_ap, out_ap, reads=(), writes=()):
        reads, writes = tuple(reads), tuple(writes)
        ex = [(("cc",), self.cnt["cc"])] if self.cnt["cc"] > 0 else []
        waits = self._deps("pool", reads, writes, ex)
        self.cnt["cc"] += 1
        token = (("cc",), self.cnt["cc"])
        self.ops["pool"].append(("k", kind, dict(alu=alu, ins=in_ap, outs=out_ap), waits, None))
        self._commit(token, reads, writes)
        return token

    def barrier(self):
        toks = [((e, (self.cnt[e] - 1) // EPOCH), (self.cnt[e] - 1) % EPOCH + 1)
                for e in ("pe", "act", "dve", "pool") if self.cnt[e] > 0]
        toks += [(sk, 16 * n) for sk, n in self.dma_cnt.items()]
        for eng in ENGS:
            waits = self._deps(eng, (), (), toks)
            if waits:
                self.ops[eng].append(("b", None, None, waits, None))

    def emit(self):
        nc = self.nc
        with contextlib.ExitStack() as st:
            sems = {}
            for e in ENGS:
                for ep in range((max(self.cnt[e], 1) - 1) // EPOCH + 1):
                    sems[(e, ep)] = st.enter_context(nc.semaphore("c_%s_%d" % (e, ep)))
            sems[("cc",)] = st.enter_context(nc.semaphore("c_cc"))
            for (sk, n) in self.dma_cnt.items():
                sems[sk] = st.enter_context(nc.semaphore("d_%s_%d" % (sk[1], sk[2])))
            block = st.enter_context(nc.Block())
            fin = {}
            for (sk, n) in self.dma_cnt.items():
                fin[sk] = 16 * n
            for e in ENGS:
                if e != "sp" and self.cnt[e] > 0:
                    fin[(e, (self.cnt[e] - 1) // EPOCH)] = (self.cnt[e] - 1) % EPOCH + 1
            if self.cnt["cc"] > 0:
                fin[("cc",)] = self.cnt["cc"]

            def run(eng_name, handle):
                for (kind, name, kw, waits, sk) in self.ops[eng_name]:
                    for (wk, val) in waits:
                        handle.wait_ge(sems[wk], val)
                    if kind == "b":
                        continue
                    if kind == "k":
                        ins = handle.collective_compute(name, kw["alu"], replica_groups=RG,
                                                        ins=[kw["ins"]], outs=[kw["outs"]])
                        ins.then_inc(sems[("cc",)])
                    else:
                        ins = getattr(handle, name)(**kw)
                        if kind == "c":
                            ins.then_inc(sems[sk], 1)
                        else:
                            ins.then_inc(sems[sk], 16)
                if eng_name == "sp":
                    for wk, val in fin.items():
                        handle.wait_ge(sems[wk], val)

            @block.tensor
            def _(e):
                run("pe", e)

            @block.scalar
            def _(e):
                run("act", e)

            @block.vector
            def _(e):
                run("dve", e)

            @block.gpsimd
            def _(e):
                run("pool", e)

            @block.sync
            def _(e):
                run("sp", e)


def build(n_layers=4, stage=None):
    nc = bass.Bass("TRN2", target_bir_lowering=False)
    P = Prog(nc)

    def din(name, shape, dt=F32):
        return nc.dram_tensor(name, list(shape), dt, kind="ExternalInput")

    def dscr(name, shape, dt=F32):
        return nc.dram_tensor(name, list(shape), dt)

    xT_in = din("xT", [D, TL])
    cT_in = din("cT", [128, 32])
    downc = din("downc", [5, D, 32])
    upc = din("upc", [26, 256, 512])
    biasc = din("biasc", [128, 104])
    lnT_in = din("lnT", [128, 16, 32])
    wr_in = din("wr", [4, D, 36])
    rb_in = din("rb", [128, 4, 4, 36])
    wqkvA = din("wqkvA", [2, 2, D, 1152])
    woA_in = din("woA", [2, 256, D])
    masks_in = din("masksA", [128, 24, 128], BF16)
    abiasA_in = din("abiasA", [128, 2, 17])
    wqB = din("wqB", [2, D, 512])
    kvw_in = din("kvw", [D, 256])
    woB_in = din("woB", [2, 512, D])
    abiasB_in = din("abiasB", [128, 4, 64])
    sel_in = din("sel", [32, 32, 128], BF16)
    wg_in = din("wg", [4, 4 * D, 256])
    wu_in = din("wu", [4, 4 * D, 256])
    wd_in = din("wd", [4, 4 * 256, D])
    identf_in = din("identf", [128, 128])
    onesf_in = din("onesf", [128, 128])
    outT = nc.dram_tensor("outT", [D, TL], F32, kind="ExternalOutput")

    r_loc = dscr("r_loc", [32, 5]); r_all = dscr("r_all", [256, 5])
    m_loc = dscr("m_loc", [128, 104]); m_all = dscr("m_all", [1024, 104])
    hm_loc = dscr("hm_loc", [D, TL], BF16); hm_all = dscr("hm_all", [NCORE * D, TL], BF16)
    hk_loc = dscr("hk_loc", [D, TL], BF16); hk_all = dscr("hk_all", [NCORE * D, TL], BF16)
    oA_loc = dscr("oA_loc", [256, S], BF16)
    oB_loc = dscr("oB_loc", [512, S], BF16)
    mixpad = dscr("mixpad", [NCORE * D, TL])
    mix_x = dscr("mix_x", [D, TL])
    wg_loc = [dscr("wg_loc%d" % i, [4 * D, 256]) for i in range(4)]
    wu_loc = [dscr("wu_loc%d" % i, [4 * D, 256]) for i in range(4)]
    wd_loc = [dscr("wd_loc%d" % i, [1024, D]) for i in range(4)]
    wg_all = [dscr("wg_all%d" % i, [32 * D, 256]) for i in range(4)]
    wu_all = [dscr("wu_all%d" % i, [32 * D, 256]) for i in range(4)]
    wd_all = [dscr("wd_all%d" % i, [8192, D]) for i in range(4)]
    y_scr = dscr("y_scr", [D, TL])
    xa_scr = dscr("xa_scr", [D, TL])
    xb_scr = dscr("xb_scr", [D, TL])

    st = contextlib.ExitStack()
    with st:
        sb = lambda n, s, d: st.enter_context(nc.sbuf_tensor(n, s, d))
        ARW = 43008
        arena = sb("arena", [128, ARW], F32)
        modT = sb("modT", [128, 8, 104], F32)
        lnT = sb("lnT_s", [128, 16, 32], F32)
        Wr = sb("Wr_s", [128, 32, 36], F32)
        rb = sb("rb_s", [128, 4, 4, 36], F32)
        identf = sb("identf_s", [128, 128], F32)
        onesf = sb("onesf_s", [128, 128], F32)
        sel = sb("sel_s", [32, 32, 128], BF16)
        abiasA = sb("abiasA_s", [128, 2, 17], F32)
        abiasB = sb("abiasB_s", [128, 4, 64], F32)
        kmT = sb("kmT", [128, 32], F32)
        small = sb("small", [128, 2048], F32)
        PS = [st.enter_context(nc.psum_tensor("ps%d" % i, [128, 512], F32)) for i in range(8)]

        def psk(i):
            return ("ps", i)

        def A32(off, n):
            return arena[:, off:off + n]

        def A16(off, n):
            return arena[:, off:off + n].bitcast(BF16)

        def mv(vec, kc):
            j = vec * 4 + (kc % 4)
            return modT[:, kc // 4, j:j + 1]

        def lnv(idx, kc):
            return lnT[:, idx, kc:kc + 1]

        P.dma("sp", lnT[:], lnT_in.ap(), writes=["lnT"])
        P.dma("sp", rb[:], rb_in.ap(), writes=["rb"])
        P.dma("sp", identf[:], identf_in.ap(), writes=["identf"])
        P.dma("sp", onesf[:], onesf_in.ap(), writes=["onesf"])
        P.dma("sp", sel[:], sel_in.ap(), writes=["sel"])
        P.dma("sp", abiasA[:], abiasA_in.ap(), writes=["abiasA"])
        P.dma("sp", abiasB[:], abiasB_in.ap(), writes=["abiasB"])

        def gather_w(src_ap, loc, allt, key, nsplit=4):
            rows = loc.shape[0]
            step = rows // nsplit
            for i in range(nsplit):
                P.dma("act", loc[i * step:(i + 1) * step, :], src_ap[i * step:(i + 1) * step, :],
                      writes=[(key, "loc", i)])
            P.coll("AllGather", ALU.bypass, loc.ap().opt(), allt.ap().opt(),
                   reads=[(key, "loc", i) for i in range(nsplit)], writes=[key])

        def gather_moe(l):
            gather_w(wg_in.ap()[l], wg_loc[l], wg_all[l], ("wg", l))
            gather_w(wu_in.ap()[l], wu_loc[l], wu_all[l], ("wu", l))
            gather_w(wd_in.ap()[l], wd_loc[l], wd_all[l], ("wd", l))

        def phase_mod():
            cs = A32(0, 32)
            scs = A32(32, 32)
            P.dma("sp", cs, cT_in.ap(), writes=["cs"])
            P.op("act", "activation", dict(out=scs, in_=cs, func=AF.Silu), reads=["cs"], writes=["scs"])
            for l in range(5):
                dw = A32(1024 + (l % 2) * 1024, 1024).rearrange("p (c j) -> p c j", c=32)
                src = downc.ap()[l].rearrange("(c p) j -> p c j", p=128)
                for h in range(2):
                    P.dma("sp", dw[:, h * 16:(h + 1) * 16, :], src[:, h * 16:(h + 1) * 16, :],
                          writes=[("dw", l % 2, h)])
                for kc in range(32):
                    P.op("pe", "matmul", dict(out=PS[0][0:32, l:l + 1], lhsT=dw[:, kc, :], rhs=scs[:, kc:kc + 1],
                                              start=(kc == 0), stop=(kc == 31)),
                         reads=[("dw", l % 2, 0), ("dw", l % 2, 1), "scs"], writes=[psk(0)])
            rl = small[0:32, 0:5]
            P.op("dve", "tensor_copy", dict(out=rl, in_=PS[0][0:32, 0:5]), writes=[psk(0), "rl"])
            P.dma("sp", r_loc.ap(), rl, reads=["rl"], writes=["r_loc"])
            P.coll("AllGather", ALU.bypass, r_loc.ap().opt(), r_all.ap().opt(), reads=["r_loc"], writes=["r_all"])
            rT = small[:, 8:18].rearrange("p (c l) -> p c l", c=2)
            P.dma("sp", rT, r_all.ap().rearrange("(c p) l -> p c l", p=128), reads=["r_all"], writes=["rT"])
            for v in range(26):
                l = min(v // 6, 4)
                ub = A32(4096 + (v % 2) * 1024, 1024).rearrange("p (c n) -> p c n", c=2)
                P.dma("sp", ub, upc.ap()[v].rearrange("(c p) n -> p c n", p=128), writes=[("ub", v % 2)])
                for ch in range(4):
                    for kc in range(2):
                        P.op("pe", "matmul", dict(out=PS[1][:, v * 4 + ch:v * 4 + ch + 1],
                                                  lhsT=ub[:, kc, ch * 128:(ch + 1) * 128], rhs=rT[:, kc, l:l + 1],
                                                  start=(kc == 0), stop=(kc == 1)),
                             reads=[("ub", v % 2), "rT"], writes=[psk(1)])
            bs = small[:, 32:136]
            ml = small[:, 160:264]
            P.dma("sp", bs, biasc.ap(), writes=["bs"])
            P.op("dve", "tensor_tensor", dict(out=ml, in0=PS[1][:, 0:104], in1=bs, op=ALU.add),
                 reads=["bs"], writes=[psk(1), "ml"])
            for v in range(26):
                if (v < 24 and v % 3 != 0) or v == 25:
                    P.op("dve", "tensor_scalar", dict(out=ml[:, v * 4:v * 4 + 4], in0=ml[:, v * 4:v * 4 + 4],
                                                      scalar1=1.0, scalar2=None, op0=ALU.add),
                         writes=["ml"])
            P.dma("sp", m_loc.ap(), ml, reads=["ml"], writes=["m_loc"])
            P.coll("AllGather", ALU.bypass, m_loc.ap().opt(), m_all.ap().opt(), reads=["m_loc"], writes=["m_all"])
            P.dma("sp", modT[:], m_all.ap().rearrange("(r p) n -> p r n", p=128), reads=["m_all"], writes=["modT"])

        def phase_hm0():
            xv = xT_in.ap().rearrange("(c p) t -> p c t", p=128)
            hv = hm_loc.ap().rearrange("(c p) t -> p c t", p=128)
            for q in range(16):
                xt = A32(8192 + (q % 2) * 2048, 2048).rearrange("p (c t) -> p c t", c=2)
                hb = A16(12288 + (q % 2) * 1024, 1024).rearrange("p (c t) -> p c t", c=2)
                P.dma("sp", xt, xv[:, 2 * q:2 * q + 2, :], writes=[("h0x", q % 2)])
                for j in range(2):
                    kc = 2 * q + j
                    P.op("dve", "tensor_scalar", dict(out=hb[:, j, :], in0=xt[:, j, :], scalar1=mv(1, kc),
                                                      scalar2=mv(0, kc), op0=ALU.mult, op1=ALU.add),
                         reads=[("h0x", q % 2), "modT"], writes=[("h0h", q % 2, j)])
                P.dma("act", hv[:, 2 * q:2 * q + 2, :], hb, reads=[("h0h", q % 2, 0), ("h0h", q % 2, 1)],
                      writes=[("hm_loc", q)])
            P.coll("AllGather", ALU.bypass, hm_loc.ap().opt(), hm_all.ap().opt(),
                   reads=[("hm_loc", q) for q in range(16)], writes=["hm_all"])

        evac_i = [0]

        def evac(out_ap, in_ap, bank, writes, reads=()):
            i = evac_i[0]
            evac_i[0] += 1
            if i % 2 == 0:
                P.op("act", "copy", dict(out=out_ap, in_=in_ap), reads=reads, writes=[psk(bank)] + list(writes))
            else:
                P.op("dve", "tensor_copy", dict(out=out_ap, in_=in_ap), reads=reads, writes=[psk(bank)] + list(writes))

        def zero_pad(pad, key):
            z = A16(0, 4096)
            P.op("pool", "memset", dict(ap=A32(0, 4096), constant=0.0), writes=["zpad"])
            rows = pad.shape[0]
            v = pad.ap().rearrange("(n p) t -> p n t", p=128)
            n = rows // 128
            for i in range(0, n, 8):
                P.dma("sp", v[:, i:i + 8, :], z.rearrange("p (n t) -> p n t", n=8), reads=["zpad"],
                      writes=[(key, "z", i)])
            return [(key, "z", i) for i in range(0, n, 8)]

        def phase_attnA(l, zkeys):
            Wh = A16(0, 18432).rearrange("p (c n) -> p c n", c=32)
            hmb = [A16(18432 + i * 4096, 4096).rearrange("p (c t) -> p c t", c=32) for i in range(2)]
            KT = A16(26624, 4608).rearrange("p (g t) -> p g t", g=3)
            Vr = A16(31232, 4680).rearrange("p (s g e) -> p s g e", s=RING, g=3)
            QT = A16(35912, 384).rearrange("p (g t) -> p g t", g=3)
            masks = A16(36296, 1536).rearrange("p (m q) -> p m q", m=24)
            Et = [A16(37832 + i * 64, 64) for i in range(4)]
            Pt = [A16(38088 + i * 64, 64) for i in range(4)]
            ob = [A32(38344 + i * 128, 128) for i in range(2)]
            oTs = [A16(38600 + i * 128, 128) for i in range(2)]
            rec = [small[:, 300 + i:301 + i] for i in range(2)]
            P.dma("sp", masks, masks_in.ap(), writes=["masks"])
            P.op("pool", "memset", dict(ap=Vr[:, :, :, 128:130], constant=1.0), writes=["Vones"])
            hmv = hm_all.ap().rearrange("(r c p) t -> p c r t", r=NCORE, p=128)
            okeys = []
            blk = 0
            for hh in range(2):
                wv = wqkvA.ap()[l, hh].rearrange("(c p) n -> p c n", p=128)
                for q4 in range(4):
                    P.dma("pool", Wh[:, q4 * 8:(q4 + 1) * 8, :], wv[:, q4 * 8:(q4 + 1) * 8, :], writes=[("Wh", q4)])
                whk = [("Wh", q4) for q4 in range(4)]
                for b in range(32):
                    hb = hmb[blk % 2]
                    hk_ = ("hmb", blk % 2)
                    blk += 1
                    r_, t0 = b // 4, (b % 4) * 256
                    for half in range(2):
                        P.dma("sp", hb[:, half * 16:(half + 1) * 16, :],
                              hmv[:, half * 16:(half + 1) * 16, r_, t0:t0 + 256],
                              reads=["hm_all"], writes=[(hk_, half)])
                    hkeys = [(hk_, 0), (hk_, 1)]
                    pi = 0
                    for g in range(3):
                        for j in range(2):
                            col0 = j * 384 + g * 128
                            bank = pi % 2
                            pi += 1
                            for c in range(32):
                                P.op("pe", "matmul", dict(out=PS[bank][:, 0:256], lhsT=Wh[:, c, col0:col0 + 128],
                                                          rhs=hb[:, c, :], start=(c == 0), stop=(c == 31)),
                                     reads=whk + hkeys, writes=[psk(bank)])
                            if j == 0:
                                evac(QT[:, g, :], PS[bank][:, 0:256], bank, [("QT", g)])
                            else:
                                s0 = (2 * b) % RING
                                evac(KT[:, g, s0 * 128:(s0 + 2) * 128], PS[bank][:, 0:256], bank,
                                     [("KT", g, s0), ("KT", g, s0 + 1)])
                    for tt in range(2):
                        slot = (2 * b + tt) % RING
                        bank = pi % 2
                        pi += 1
                        for c in range(32):
                            P.op("pe", "matmul", dict(out=PS[bank][:, 0:384], lhsT=hb[:, c, tt * 128:(tt + 1) * 128],
                                                      rhs=Wh[:, c, 768:1152], start=(c == 0), stop=(c == 31)),
                                 reads=whk + hkeys, writes=[psk(bank)])
                        evac(Vr[:, slot, :, 0:128], PS[bank][:, 0:384].rearrange("p (g e) -> p g e", g=3), bank,
                             [("V", slot)])
                    chunks = []
                    for tt in range(2):
                        qt = 2 * b + tt
                        lst = []
                        for g in range(3):
                            for dt in range(NREL[g] - 1, -1, -1):
                                if qt - dt >= 0:
                                    lst.append((tt, g, dt, qt - dt))
                        for i, ci in enumerate(lst):
                            chunks.append(ci + (i == 0, i == len(lst) - 1))
                    n = len(chunks)
                    LOOK = 2
                    osi = blk % 2
                    for i in range(n + LOOK):
                        if i < n:
                            tt, g, dt, kt, first, last = chunks[i]
                            slot = kt % RING
                            si = i % 4
                            psi = 2 + i % 3
                            P.op("pe", "matmul", dict(out=PS[psi][:, 0:128], lhsT=KT[:, g, slot * 128:(slot + 1) * 128],
                                                      rhs=QT[:, g, tt * 128:(tt + 1) * 128], start=True, stop=True),
                                 reads=[("KT", g, slot), ("QT", g)], writes=[psk(psi)])
                            P.op("act", "activation", dict(out=Et[si], in_=PS[psi][:, 0:128], func=AF.Exp,
                                                           bias=abiasA[:, hh, dt:dt + 1], scale=SCALE),
                                 reads=["abiasA"], writes=[psk(psi), ("Et", si)])
                            P.op("dve", "tensor_tensor", dict(out=Pt[si], in0=Et[si], in1=masks[:, GOFF[g] + dt, :],
                                                              op=ALU.mult),
                                 reads=[("Et", si), "masks"], writes=[("Pt", si)])
                        if i >= LOOK:
                            tt, g, dt, kt, first, last = chunks[i - LOOK]
                            slot = kt % RING
                            si = (i - LOOK) % 4
                            oi = tt
                            P.op("pe", "matmul", dict(out=PS[5 + oi][:, 0:130], lhsT=Pt[si], rhs=Vr[:, slot, g, :],
                                                      start=first, stop=last),
                                 reads=[("Pt", si), ("V", slot), "Vones"], writes=[psk(5 + oi)])
                            if last:
                                P.op("dve", "reciprocal", dict(out=rec[oi], in_=PS[5 + oi][:, 128:129]),
                                     writes=[psk(5 + oi), ("rec", oi)])
                                P.op("dve", "tensor_scalar", dict(out=ob[oi], in0=PS[5 + oi][:, 0:128],
                                                                  scalar1=rec[oi], scalar2=None, op0=ALU.mult),
                                     reads=[("rec", oi)], writes=[psk(5 + oi), ("ob", oi)])
                                P.op("pe", "matmul", dict(out=PS[7][:, 0:128], lhsT=ob[oi], rhs=identf[:],
                                                          start=True, stop=True),
                                     reads=[("ob", oi), "identf"], writes=[psk(7)])
                                P.op("act", "copy", dict(out=oTs[osi][:, tt * 128:(tt + 1) * 128], in_=PS[7][:, 0:128]),
                                     writes=[psk(7), ("oTs", osi, tt)])
                    P.dma("act", oA_loc.ap()[hh * 128:(hh + 1) * 128, b * 256:(b + 1) * 256],
                          oTs[osi], reads=[("oTs", osi, 0), ("oTs", osi, 1)], writes=[("o_loc", hh, b // 2)])
                    okeys.append(("o_loc", hh, b // 2))
            return okeys

        def phase_wo(KC, o_loc, wo_src):
            Wo = A16(0, KC * 2048).rearrange("p (c f) -> p c f", c=KC)
            for kc in range(KC):
                P.dma("pool", Wo[:, kc, :], wo_src[kc * 128:(kc + 1) * 128, :], writes=[("Wo", kc)])
            wok = [("Wo", kc) for kc in range(KC)]
            ov = o_loc.ap().rearrange("(c p) t -> p c t", p=128)
            padv = mixpad.ap().rearrange("(r f) t -> f r t", r=NCORE)
            pk = []
            for tb in range(16):
                ob_ = A16(8192 + (tb % 2) * 1024, KC * 256).rearrange("p (c t) -> p c t", c=KC)
                P.dma("sp", ob_, ov[:, :, tb * 512:(tb + 1) * 512], reads=[("o_loc", c, tb) for c in range(KC)],
                      writes=[("wo_ob", tb % 2)])
                for fc in range(32):
                    bank = fc % 2
                    for kc in range(KC):
                        P.op("pe", "matmul", dict(out=PS[bank][:, 0:512], lhsT=Wo[:, kc, fc * 128:(fc + 1) * 128],
                                                  rhs=ob_[:, kc, :], start=(kc == 0), stop=(kc == KC - 1)),
                             reads=wok + [("wo_ob", tb % 2)], writes=[psk(bank)])
                    si = fc % 4
                    stg = A32(10240 + si * 512, 512)
                    evac(stg, PS[bank][:, 0:512], bank, [("wo_stg", si)])
                    P.dma("act", padv[fc * 128:(fc + 1) * 128, tb // 2, (tb % 2) * 512:(tb % 2) * 512 + 512], stg,
                          reads=[("wo_stg", si)], writes=[("mixpad", tb, fc)])
                    pk.append(("mixpad", tb, fc))
            P.coll("ReduceScatter", ALU.add, mixpad.ap().opt(), mix_x.ap().opt(), reads=pk, writes=["mix_x"])

        T = [A32(36864 + i * 512, 512) for i in range(12)]

        def ln_accum(fc, ybank, gate_vec, x_src, t0):
            xt = T[0 + fc % 2]
            t1 = T[2 + fc % 2]
            yt = T[4 + fc % 2]
            sq = T[6 + fc % 2]
            P.dma("sp", xt, x_src.ap()[fc * 128:(fc + 1) * 128, t0:t0 + 512], reads=[("xsrc", x_src.name, fc)],
                  writes=[("T", 0 + fc % 2)])
            if ybank is None:
                P.dma("sp", t1, mix_x.ap()[fc * 128:(fc + 1) * 128, t0:t0 + 512], reads=["mix_x"],
                      writes=[("T", 2 + fc % 2)])
                P.op("dve", "tensor_scalar", dict(out=t1, in0=t1, scalar1=mv(gate_vec, fc), scalar2=None,
                                                  op0=ALU.mult), reads=["modT"], writes=[("T", 2 + fc % 2)])
            else:
                P.op("dve", "tensor_scalar", dict(out=t1, in0=PS[ybank][:, 0:512], scalar1=mv(gate_vec, fc),
                                                  scalar2=None, op0=ALU.mult), reads=["modT"],
                     writes=[psk(ybank), ("T", 2 + fc % 2)])
            P.op("act", "mul", dict(out=yt, in_=xt, mul=ALPHA), reads=[("T", 0 + fc % 2)], writes=[("T", 4 + fc % 2)])
            P.op("pool", "tensor_tensor", dict(out=yt, in0=yt, in1=t1, op=ALU.add),
                 reads=[("T", 2 + fc % 2)], writes=[("T", 4 + fc % 2)])
            P.op("act", "square", dict(out=sq, in_=yt), reads=[("T", 4 + fc % 2)], writes=[("T", 6 + fc % 2)])
            P.op("pe", "matmul", dict(out=PS[2][:, 0:512], lhsT=onesf[:], rhs=yt, start=(fc == 0), stop=(fc == 31)),
                 reads=["onesf", ("T", 4 + fc % 2)], writes=[psk(2)])
            P.op("pe", "matmul", dict(out=PS[3][:, 0:512], lhsT=onesf[:], rhs=sq, start=(fc == 0), stop=(fc == 31)),
                 reads=["onesf", ("T", 6 + fc % 2)], writes=[psk(3)])
            P.dma("act", y_scr.ap()[fc * 128:(fc + 1) * 128, t0:t0 + 512], yt, reads=[("T", 4 + fc % 2)],
                  writes=[("y_scr", fc)])

        def ln_stats():
            mean, rstd, msq = T[8], T[9], T[10]
            P.op("act", "mul", dict(out=mean, in_=PS[2][:, 0:512], mul=1.0 / D), writes=[psk(2), ("T", 8)])
            P.op("act", "mul", dict(out=rstd, in_=PS[3][:, 0:512], mul=1.0 / D), writes=[psk(3), ("T", 9)])
            P.op("dve", "tensor_tensor", dict(out=msq, in0=mean, in1=mean, op=ALU.mult), reads=[("T", 8)],
                 writes=[("T", 10)])
            P.op("dve", "tensor_tensor", dict(out=rstd, in0=rstd, in1=msq, op=ALU.subtract), reads=[("T", 10)],
                 writes=[("T", 9)])
            P.op("dve", "tensor_scalar", dict(out=rstd, in0=rstd, scalar1=LN_EPS, scalar2=None, op0=ALU.add),
                 writes=[("T", 9)])
            P.op("act", "sqrt", dict(out=rstd, in_=rstd), writes=[("T", 9)])
            P.op("dve", "reciprocal", dict(out=rstd, in_=rstd), writes=[("T", 9)])

        def ln_apply(fc, t0, ln_idx, x_dst, derived):
            mean, rstd = T[8], T[9]
            yt = T[0 + fc % 2]
            xn = T[2 + fc % 2]
            P.dma("sp", yt, y_scr.ap()[fc * 128:(fc + 1) * 128, t0:t0 + 512], reads=[("y_scr", fc)],
                  writes=[("T", 0 + fc % 2)])
            P.op("dve", "tensor_tensor", dict(out=yt, in0=yt, in1=mean, op=ALU.subtract), reads=[("T", 8)],
                 writes=[("T", 0 + fc % 2)])
            P.op("pool", "tensor_tensor", dict(out=yt, in0=yt, in1=rstd, op=ALU.mult), reads=[("T", 9)],
                 writes=[("T", 0 + fc % 2)])
            P.op("dve", "tensor_scalar", dict(out=xn, in0=yt, scalar1=lnv(ln_idx * 2, fc), scalar2=lnv(ln_idx * 2 + 1, fc),
                                              op0=ALU.mult, op1=ALU.add),
                 reads=[("T", 0 + fc % 2), "lnT"], writes=[("T", 2 + fc % 2)])
            P.dma("act", x_dst.ap()[fc * 128:(fc + 1) * 128, t0:t0 + 512], xn, reads=[("T", 2 + fc % 2)],
                  writes=[("xsrc", x_dst.name, fc)])
            for di, (shv, scv, fn) in enumerate(derived):
                dtile = T[4 + 2 * di + fc % 2]
                eng = "dve"
                P.op(eng, "tensor_scalar", dict(out=dtile, in0=xn, scalar1=mv(scv, fc), scalar2=mv(shv, fc),
                                                op0=ALU.mult, op1=ALU.add),
                     reads=[("T", 2 + fc % 2), "modT"], writes=[("T", 4 + 2 * di + fc % 2)])
                fn(fc, dtile, ("T", 4 + 2 * di + fc % 2))

        def phase_tp(l, last):
            x_in = xT_in if l == 0 else xb_scr
            x_out = outT if last else xb_scr
            Bb = A16(0, 16384).rearrange("p (c t) -> p c t", c=64)
            HF = A16(16384, 8192).rearrange("p (c t) -> p c t", c=32)
            Wb = [24576 + i * 4096 for i in range(3)]
            vb = 6 * l
            P.dma("sp", Wr[:], wr_in.ap()[l].rearrange("(c p) n -> p c n", p=128), writes=["Wr"])
            wgv = wg_all[l].ap().rearrange("(e c p) j -> p e c j", e=32, p=128)
            wuv = wu_all[l].ap().rearrange("(e c p) j -> p e c j", e=32, p=128)
            wdv = wd_all[l].ap().rearrange("(c p) f -> p c f", p=128)
            hmv_ = hm_loc.ap()
            hkv_ = hk_loc.ap()
            wi = [0]
            for tg in range(2):
                t0 = tg * 512
                for fc in range(32):
                    ln_accum(fc, None, vb + 2, x_in, t0)
                ln_stats()

                def d_hf(fc, ap, key):
                    P.op("act", "copy", dict(out=HF[:, fc, :], in_=ap), reads=[key], writes=[("HF", fc)])
                    P.op("pe", "matmul", dict(out=PS[4][0:36, 0:512], lhsT=Wr[:, fc, :], rhs=ap, start=(fc == 0),
                                              stop=(fc == 31)), reads=["Wr", key], writes=[psk(4)])
                for fc in range(32):
                    ln_apply(fc, t0, l * 2, xa_scr, [(vb + 3, vb + 4, d_hf)])
                lt = T[6]
                P.op("act", "copy", dict(out=lt[0:36, :], in_=PS[4][0:36, 0:512]), writes=[psk(4), ("T", 6)])
                for tt in range(4):
                    P.op("pe", "matmul", dict(out=PS[5][:, tt * 36:(tt + 1) * 36], lhsT=lt[0:36, tt * 128:(tt + 1) * 128],
                                              rhs=identf[0:36, 0:36], start=True, stop=True),
                         reads=[("T", 6), "identf"], writes=[psk(5)])
                L = T[7][:, 0:144].rearrange("p (t n) -> p t n", t=4)
                P.op("dve", "tensor_tensor", dict(out=L, in0=PS[5][:, 0:144].rearrange("p (t n) -> p t n", t=4),
                                                  in1=rb[:, l, :, :], op=ALU.add), reads=["rb"], writes=[psk(5), ("T", 7)])
                sm = small[:, 320:1024]
                gmax = sm[:, 0:4]
                gd = sm[:, 4:20].rearrange("p (t g) -> p t g", t=4)
                ge = sm[:, 20:36].rearrange("p (t g) -> p t g", t=4)
                pg = sm[:, 36:40]
                goh = sm[:, 40:56].rearrange("p (t g) -> p t g", t=4)
                Em = sm[:, 56:184].rearrange("p (t g e) -> p t g e", t=4, g=4)
                M8 = sm[:, 184:216].rearrange("p (t e) -> p t e", t=4)
                dd = sm[:, 216:220]
                ed = sm[:, 220:224]
                w1 = sm[:, 224:228]
                w2 = sm[:, 228:232]
                Wt = sm[:, 232:360].rearrange("p (t e) -> p t e", t=4)
                Wt2 = sm[:, 360:488].rearrange("p (t e) -> p t e", t=4)
                rk = ["rt"]
                G = L[:, :, 0:4]
                P.op("dve", "tensor_reduce", dict(out=gmax, in_=G, axis=AX.X, op=ALU.max), reads=[("T", 7)], writes=rk)
                P.op("dve", "tensor_tensor", dict(out=gd, in0=G, in1=gmax.unsqueeze(2).to_broadcast([128, 4, 4]),
                                                  op=ALU.subtract), reads=[("T", 7)], writes=rk)
                P.op("act", "activation", dict(out=ge, in_=gd, func=AF.Exp), writes=rk)
                P.op("dve", "tensor_reduce", dict(out=pg, in_=ge, axis=AX.X, op=ALU.add), writes=rk)
                P.op("dve", "reciprocal", dict(out=pg, in_=pg), writes=rk)
                P.op("dve", "tensor_scalar", dict(out=goh, in0=gd, scalar1=0.0, scalar2=None, op0=ALU.is_equal), writes=rk)
                P.op("dve", "tensor_scalar", dict(out=goh, in0=goh, scalar1=-1.0, scalar2=1e30, op0=ALU.add,
                                                  op1=ALU.mult), writes=rk)
                P.op("dve", "tensor_tensor", dict(out=Em, in0=L[:, :, 4:36].rearrange("p t (g e) -> p t g e", g=4),
                                                  in1=goh.unsqueeze(3).to_broadcast([128, 4, 4, 8]), op=ALU.add),
                     reads=[("T", 7)], writes=rk)
                Emf = sm[:, 56:184].rearrange("p (t n) -> p t n", t=4)
                for tt in range(4):
                    P.op("dve", "max", dict(out=M8[:, tt, :], in_=Emf[:, tt, :]), writes=rk)
                P.op("dve", "tensor_tensor", dict(out=dd, in0=M8[:, :, 1], in1=M8[:, :, 0], op=ALU.subtract), writes=rk)
                P.op("act", "activation", dict(out=ed, in_=dd, func=AF.Exp), writes=rk)
                P.op("dve", "tensor_scalar", dict(out=w1, in0=ed, scalar1=1.0, scalar2=None, op0=ALU.add), writes=rk)
                P.op("dve", "reciprocal", dict(out=w1, in_=w1), writes=rk)
                P.op("dve", "tensor_tensor", dict(out=w1, in0=w1, in1=pg, op=ALU.mult), writes=rk)
                P.op("dve", "tensor_tensor", dict(out=w2, in0=w1, in1=ed, op=ALU.mult), writes=rk)
                for tt in range(4):
                    P.op("dve", "tensor_scalar", dict(out=Wt[:, tt, :], in0=Emf[:, tt, :], scalar1=M8[:, tt, 0:1],
                                                      scalar2=w1[:, tt:tt + 1], op0=ALU.is_equal, op1=ALU.mult), writes=rk)
                    P.op("dve", "tensor_scalar", dict(out=Wt2[:, tt, :], in0=Emf[:, tt, :], scalar1=M8[:, tt, 1:2],
                                                      scalar2=w2[:, tt:tt + 1], op0=ALU.is_equal, op1=ALU.mult), writes=rk)
                P.op("dve", "tensor_tensor", dict(out=Wt, in0=Wt, in1=Wt2, op=ALU.add), writes=rk)
                for tt in range(4):
                    P.op("pe", "matmul", dict(out=PS[5][0:32, tt * 128:(tt + 1) * 128], lhsT=Wt[:, tt, :], rhs=identf[:],
                                              start=True, stop=True), reads=rk + ["identf"], writes=[psk(5)])
                WTf = T[10]
                WTh = T[11][:, 0:256].bitcast(BF16)
                WTl = T[11][:, 256:512].bitcast(BF16)
                WThf = T[7]
                P.op("act", "copy", dict(out=WTf[0:32, :], in_=PS[5][0:32, 0:512]), writes=[psk(5), ("T", 10)])
                P.op("dve", "tensor_copy", dict(out=WTh[0:32, :], in_=WTf[0:32, :]), reads=[("T", 10)], writes=["WTh"])
                P.op("dve", "tensor_copy", dict(out=WThf[0:32, :], in_=WTh[0:32, :]), reads=["WTh"], writes=[("T", 7)])
                P.op("dve", "tensor_tensor", dict(out=WTl[0:32, :], in0=WTf[0:32, :], in1=WThf[0:32, :],
                                                  op=ALU.subtract), reads=[("T", 10), ("T", 7)], writes=["WTl"])
                for e in range(32):
                    wts = []
                    for (wv_, wkey) in ((wgv, ("wg", l)), (wuv, ("wu", l))):
                        wslot = wi[0] % 3
                        wi[0] += 1
                        wt = A16(Wb[wslot], 4096).rearrange("p (c j) -> p c j", c=32)
                        for h in range(2):
                            P.dma("pool", wt[:, h * 16:(h + 1) * 16, :], wv_[:, e, h * 16:(h + 1) * 16, :],
                                  reads=[wkey], writes=[("W", wslot, h)])
                        wts.append((wt, wslot))
                    wbb = 6 + e % 2
                    P.op("pe", "matmul", dict(out=PS[wbb][:, 0:512], lhsT=sel[:, e, :], rhs=WTh[0:32, :], start=True,
                                              stop=False), reads=["sel", "WTh"], writes=[psk(wbb)])
                    P.op("pe", "matmul", dict(out=PS[wbb][:, 0:512], lhsT=sel[:, e, :], rhs=WTl[0:32, :], start=False,
                                              stop=True), reads=["sel", "WTl"], writes=[psk(wbb)])
                    wBs = T[8 + e % 2]
                    P.op("act", "copy", dict(out=wBs, in_=PS[wbb][:, 0:512]), writes=[psk(wbb), ("T", 8 + e % 2)])
                    for jh in range(2):
                        u = 2 * e + jh
                        gb, ub_ = u % 2, 2 + u % 2
                        for (bank, (wt, wslot)) in ((gb, wts[0]), (ub_, wts[1])):
                            for kc in range(32):
                                P.op("pe", "matmul", dict(out=PS[bank][:, 0:512], lhsT=wt[:, kc, jh * 128:(jh + 1) * 128],
                                                          rhs=HF[:, kc, :], start=(kc == 0), stop=(kc == 31)),
                                     reads=[("W", wslot, 0), ("W", wslot, 1), ("HF", kc)], writes=[psk(bank)])
                        sg = T[0 + u % 2]
                        tt_ = T[2 + u % 2]
                        P.op("act", "activation", dict(out=sg, in_=PS[gb][:, 0:512], func=AF.Silu),
                             writes=[psk(gb), ("T", 0 + u % 2)])
                        P.op("dve", "tensor_tensor", dict(out=tt_, in0=sg, in1=PS[ub_][:, 0:512], op=ALU.mult),
                             reads=[("T", 0 + u % 2)], writes=[psk(ub_), ("T", 2 + u % 2)])
                        P.op("pool", "tensor_tensor", dict(out=Bb[:, u, :], in0=tt_, in1=wBs, op=ALU.mult),
                             reads=[("T", 2 + u % 2), ("T", 8 + e % 2)], writes=[("Bb", u)])
                for fc in range(32):
                    wslot = wi[0] % 3
                    wi[0] += 1
                    wt = A16(Wb[wslot], 4096).rearrange("p (c f) -> p c f", c=64)
                    for h in range(4):
                        P.dma("pool", wt[:, h * 16:(h + 1) * 16, :], wdv[:, h * 16:(h + 1) * 16, fc * 128:(fc + 1) * 128],
                              reads=[("wd", l)], writes=[("W", wslot, h % 2)] if h >= 2 else [("W", wslot, h % 2)])
                    yb = fc % 2
                    for hc in range(64):
                        P.op("pe", "matmul", dict(out=PS[yb][:, 0:512], lhsT=wt[:, hc, :], rhs=Bb[:, hc, :],
                                                  start=(hc == 0), stop=(hc == 63)),
                             reads=[("W", wslot, 0), ("W", wslot, 1), ("Bb", hc)], writes=[psk(yb)])
                    ln_accum(fc, yb, vb + 5, xa_scr, t0)
                ln_stats()
                derived = []
                if not last:
                    nvb = 6 * (l + 1)

                    def d_hm(fc, ap, key, tg=tg):
                        hb = small[:, 1024 + (fc % 2) * 256:1280 + (fc % 2) * 256].bitcast(BF16)
                        P.op("act", "copy", dict(out=hb, in_=ap), reads=[key], writes=[("stg_hm", fc % 2)])
                        P.dma("act", hmv_[fc * 128:(fc + 1) * 128, tg * 512:(tg + 1) * 512], hb,
                              reads=[("stg_hm", fc % 2)], writes=[("hm_loc", tg, fc)])
                    derived.append((nvb + 0, nvb + 1, d_hm))
                    if l == 1:
                        def d_hk(fc, ap, key, tg=tg):
                            hb = small[:, 1536 + (fc % 2) * 256:1792 + (fc % 2) * 256].bitcast(BF16)
                            P.op("act", "copy", dict(out=hb, in_=ap), reads=[key], writes=[("stg_hk", fc % 2)])
                            P.dma("act", hkv_[fc * 128:(fc + 1) * 128, tg * 512:(tg + 1) * 512], hb,
                                  reads=[("stg_hk", fc % 2)], writes=[("hk_loc", tg, fc)])
                        derived.append((24, 25, d_hk))
                for fc in range(32):
                    ln_apply(fc, t0, l * 2 + 1, x_out, derived)
            if not last:
                P.coll("AllGather", ALU.bypass, hm_loc.ap().opt(), hm_all.ap().opt(),
                       reads=[("hm_loc", tg, fc) for tg in range(2) for fc in range(32)], writes=["hm_all"])
                if l == 1:
                    P.coll("AllGather", ALU.bypass, hk_loc.ap().opt(), hk_all.ap().opt(),
                           reads=[("hk_loc", tg, fc) for tg in range(2) for fc in range(32)], writes=["hk_all"])

        KTb_off, Vb_off = 0, 4096
        def moba_views():
            KTb = A16(KTb_off, 4096)
            Vb = A16(Vb_off, 4160).rearrange("p (s e) -> p s e", s=64)
            return KTb, Vb

        def phase_kv():
            KTb, Vb = moba_views()
            Wk = A16(8256, 4096).rearrange("p (c n) -> p c n", c=32)
            hmb = [A16(12352 + i * 4096, 4096).rearrange("p (c t) -> p c t", c=32) for i in range(2)]
            kv_v = kvw_in.ap().rearrange("(c p) n -> p c n", p=128)
            for h in range(2):
                P.dma("pool", Wk[:, h * 16:(h + 1) * 16, :], kv_v[:, h * 16:(h + 1) * 16, :], writes=[("Wk", h)])
            P.op("pool", "memset", dict(ap=Vb[:, :, 128:130], constant=1.0), writes=["Vbones"])
            hkv = hk_all.ap().rearrange("(r c p) t -> p c r t", r=NCORE, p=128)
            ksum = small[:, 304:305]
            for b in range(32):
                hb = hmb[b % 2]
                r_, t0 = b // 4, (b % 4) * 256
                for half in range(2):
                    P.dma("sp", hb[:, half * 16:(half + 1) * 16, :], hkv[:, half * 16:(half + 1) * 16, r_, t0:t0 + 256],
                          reads=["hk_all"], writes=[("hkb", b % 2, half)])
                hkeys = [("hkb", b % 2, 0), ("hkb", b % 2, 1), ("Wk", 0), ("Wk", 1)]
                for c in range(32):
                    P.op("pe", "matmul", dict(out=PS[0][:, 0:256], lhsT=Wk[:, c, 0:128], rhs=hb[:, c, :],
                                              start=(c == 0), stop=(c == 31)), reads=hkeys, writes=[psk(0)])
                P.op("act", "copy", dict(out=KTb[:, b * 256:(b + 1) * 256], in_=PS[0][:, 0:256]),
                     writes=[psk(0), ("KTb", b)])
                P.op("dve", "tensor_reduce", dict(out=kmT[:, b:b + 1], in_=PS[0][:, 0:256], axis=AX.X, op=ALU.add),
                     writes=[psk(0), "kmT"])
                for tt in range(2):
                    bank = 1 + tt
                    for c in range(32):
                        P.op("pe", "matmul", dict(out=PS[bank][:, 0:128], lhsT=hb[:, c, tt * 128:(tt + 1) * 128],
                                                  rhs=Wk[:, c, 128:256], start=(c == 0), stop=(c == 31)),
                             reads=hkeys, writes=[psk(bank)])
                    evac(Vb[:, 2 * b + tt, 0:128], PS[bank][:, 0:128], bank, [("Vb", 2 * b + tt)])
            P.op("dve", "tensor_scalar", dict(out=kmT[:], in0=kmT[:], scalar1=1.0 / 256.0, scalar2=None, op0=ALU.mult),
                 writes=["kmT"])

        def phase_moba(j, zkeys):
            KTb, Vb = moba_views()
            Wq = A16(8256, 8192).rearrange("p (c n) -> p c n", c=32)
            hmb = [A16(16448 + i * 4096, 4096).rearrange("p (c t) -> p c t", c=32) for i in range(2)]
            QTb = A16(24640, 128)
            QTf = A32(24768, 256)
            Pb = [A16(25024 + i * 128, 128) for i in range(4)]
            Pm = [A16(25536 + i * 64, 64) for i in range(4)]
            cmask = A16(25792, 64)
            gsb = [A32(25856 + i * 32, 32) for i in range(2)]
            sbias = [A32(25920 + i * 32, 32) for i in range(2)]
            sbT = [A16(25984 + i * 128, 128) for i in range(2)]
            ob = [A32(26240 + i * 128, 128) for i in range(2)]
            oTs = [A16(26496 + i * 128, 128) for i in range(2)]
            m8 = [small[:, 306 + 8 * i:314 + 8 * i] for i in range(2)]
            rec = [small[:, 300 + i:301 + i] for i in range(2)]
            P.dma("sp", cmask, masks_in.ap()[:, 0, :], writes=["cmask"])
            wqv = wqB.ap()[j].rearrange("(c p) n -> p c n", p=128)
            for q4 in range(4):
                P.dma("pool", Wq[:, q4 * 8:(q4 + 1) * 8, :], wqv[:, q4 * 8:(q4 + 1) * 8, :], writes=[("Wq", q4)])
            wqk = [("Wq", q4) for q4 in range(4)]
            hmv = hm_all.ap().rearrange("(r c p) t -> p c r t", r=NCORE, p=128)
            okeys = []
            ei = [0]
            for b in range(32):
                hb = hmb[b % 2]
                r_, t0 = b // 4, (b % 4) * 256
                for half in range(2):
                    P.dma("sp", hb[:, half * 16:(half + 1) * 16, :], hmv[:, half * 16:(half + 1) * 16, r_, t0:t0 + 256],
                          reads=["hm_all"], writes=[("hmb", b % 2, half)])
                hkeys = [("hmb", b % 2, 0), ("hmb", b % 2, 1)]
                for hq in range(4):
                    for c in range(32):
                        P.op("pe", "matmul", dict(out=PS[0][:, 0:256], lhsT=Wq[:, c, hq * 128:(hq + 1) * 128],
                                                  rhs=hb[:, c, :], start=(c == 0), stop=(c == 31)),
                             reads=wqk + hkeys, writes=[psk(0)])
                    P.op("act", "copy", dict(out=QTb, in_=PS[0][:, 0:256]), writes=[psk(0), "QTb"])
                    P.op("dve", "tensor_copy", dict(out=QTf, in_=PS[0][:, 0:256]), writes=[psk(0), "QTf"])
                    nb = b
                    if nb > 0:
                        for tt in range(2):
                            P.op("pe", "matmul", dict(out=PS[7][:, tt * 32:tt * 32 + 32], lhsT=QTf[:, tt * 128:(tt + 1) * 128],
                                                      rhs=kmT[:], start=True, stop=True),
                                 reads=["QTf", "kmT"], writes=[psk(7)])
                        for tt in range(2):
                            g_ = gsb[tt]
                            P.op("dve", "memset", dict(ap=g_, constant=-3.0e38), writes=[("gsb", tt)])
                            P.op("dve", "tensor_copy", dict(out=g_[:, 0:nb], in_=PS[7][:, tt * 32:tt * 32 + nb]),
                                 writes=[psk(7), ("gsb", tt)])
                            if nb > 3:
                                P.op("dve", "max", dict(out=m8[tt], in_=g_), reads=[("gsb", tt)], writes=[("m8", tt)])
                                P.op("dve", "tensor_scalar", dict(out=sbias[tt], in0=g_, scalar1=m8[tt][:, 2:3],
                                                                  scalar2=-30000.0, op0=ALU.is_lt, op1=ALU.mult),
                                     reads=[("m8", tt), ("gsb", tt)], writes=[("sbias", tt)])
                            else:
                                P.op("dve", "memset", dict(ap=sbias[tt], constant=0.0), writes=[("sbias", tt)])
                            P.op("pe", "matmul", dict(out=PS[7][0:32, 128 + tt * 128:256 + tt * 128], lhsT=sbias[tt],
                                                      rhs=identf[:], start=True, stop=True),
                                 reads=[("sbias", tt), "identf"], writes=[psk(7)])
                        P.op("act", "copy", dict(out=sbT[0][0:32, :], in_=PS[7][0:32, 128:384]), writes=[psk(7), "sbT"])
                    units = []
                    for kt in range(2 * nb):
                        units.append(("past", kt))
                    units += [("own", 2 * b, 0, True), ("own", 2 * b, 1, False), ("own", 2 * b + 1, 1, True)]
                    nun = len(units)
                    firsts = {0: True, 1: True}
                    pend = []
                    for ui, un in enumerate(units):
                        sbk = 1 + ui % 2
                        pslot = ui % 4
                        if un[0] == "past":
                            kt = un[1]
                            P.op("pe", "matmul", dict(out=PS[sbk][:, 0:256], lhsT=KTb[:, kt * 128:(kt + 1) * 128], rhs=QTb,
                                                      start=True, stop=False), reads=[("KTb", kt // 2), "QTb"],
                                 writes=[psk(sbk)])
                            P.op("pe", "matmul", dict(out=PS[sbk][:, 0:256], lhsT=sel[:, kt // 2, :], rhs=sbT[0][0:32, :],
                                                      start=False, stop=True), reads=["sel", "sbT"], writes=[psk(sbk)])
                            for tt in range(2):
                                dt = 2 * b + tt - kt
                                P.op("act", "activation", dict(out=Pb[pslot][:, tt * 128:(tt + 1) * 128],
                                                               in_=PS[sbk][:, tt * 128:(tt + 1) * 128], func=AF.Exp,
                                                               bias=abiasB[:, hq, dt:dt + 1], scale=SCALE),
                                     reads=["abiasB"], writes=[psk(sbk), ("Pb", pslot, tt)])
                            pend.append([(tt, Pb[pslot][:, tt * 128:(tt + 1) * 128], ("Pb", pslot, tt), kt, ui) for tt in range(2)])
                        else:
                            _, kt, tt, diag = un
                            dt = 2 * b + tt - kt
                            P.op("pe", "matmul", dict(out=PS[sbk][:, 0:128], lhsT=KTb[:, kt * 128:(kt + 1) * 128],
                                                      rhs=QTb[:, tt * 128:(tt + 1) * 128], start=True, stop=True),
                                 reads=[("KTb", kt // 2), "QTb"], writes=[psk(sbk)])
                            P.op("act", "activation", dict(out=Pb[pslot][:, 0:128], in_=PS[sbk][:, 0:128], func=AF.Exp,
                                                           bias=abiasB[:, hq, dt:dt + 1], scale=SCALE),
                                 reads=["abiasB"], writes=[psk(sbk), ("Pb", pslot, 0)])
                            if diag:
                                P.op("dve", "tensor_tensor", dict(out=Pm[pslot], in0=Pb[pslot][:, 0:128], in1=cmask,
                                                                  op=ALU.mult), reads=[("Pb", pslot, 0), "cmask"],
                                     writes=[("Pm", pslot)])
                                pend.append([(tt, Pm[pslot], ("Pm", pslot), kt, ui)])
                            else:
                                pend.append([(tt, Pb[pslot][:, 0:128], ("Pb", pslot, 0), kt, ui)])
                        while len(pend) > 1 or (ui == nun - 1 and pend):
                            for (tt, pap, pkey, kt, uidx) in pend.pop(0):
                                is_last = (uidx == (nun - 3 if tt == 0 else nun - 1))
                                P.op("pe", "matmul", dict(out=PS[3 + tt][:, 0:130], lhsT=pap, rhs=Vb[:, kt, :],
                                                          start=firsts[tt], stop=is_last),
                                     reads=[pkey, ("Vb", kt), "Vbones"], writes=[psk(3 + tt)])
                                firsts[tt] = False
                    osi = ei[0] % 2
                    ei[0] += 1
                    for tt in range(2):
                        P.op("dve", "reciprocal", dict(out=rec[tt], in_=PS[3 + tt][:, 128:129]),
                             writes=[psk(3 + tt), ("rec", tt)])
                        P.op("dve", "tensor_scalar", dict(out=ob[tt], in0=PS[3 + tt][:, 0:128], scalar1=rec[tt],
                                                          scalar2=None, op0=ALU.mult),
                             reads=[("rec", tt)], writes=[psk(3 + tt), ("ob", tt)])
                        P.op("pe", "matmul", dict(out=PS[5 + tt][:, 0:128], lhsT=ob[tt], rhs=identf[:], start=True, stop=True),
                             reads=[("ob", tt), "identf"], writes=[psk(5 + tt)])
                        P.op("act", "copy", dict(out=oTs[osi][:, tt * 128:(tt + 1) * 128], in_=PS[5 + tt][:, 0:128]),
                             writes=[psk(5 + tt), ("oTs", osi, tt)])
                    P.dma("act", oB_loc.ap()[hq * 128:(hq + 1) * 128, b * 256:(b + 1) * 256], oTs[osi],
                          reads=[("oTs", osi, 0), ("oTs", osi, 1)], writes=[("o_loc", hq, b // 2)])
                    okeys.append(("o_loc", hq, b // 2))
            return okeys

        def finish_stage():
            P.barrier()
            if stage == 1:
                P.dma("sp", outT.ap()[0:128, 0:832], modT[:].rearrange("p r n -> p (r n)"), reads=["modT"])
                P.dma("pool", outT.ap()[128:128 + 1024, :], hm_all.ap()[0:1024, :], reads=["hm_all"])
            elif stage == 2:
                P.dma("pool", outT.ap()[0:256, :], oA_loc.ap()[:, 0:1024],
                      reads=[("o_loc", c, tb) for c in range(2) for tb in range(16)])
            elif stage == 3:
                P.dma("sp", outT.ap(), mix_x.ap(), reads=["mix_x"])
            P.emit()

        phase_mod()
        P.barrier()
        if stage != 1:
            gather_moe(0)
        phase_hm0()
        P.barrier()
        if stage == 1:
            finish_stage()
            return nc
        for l in range(n_layers):
            if l + 1 < n_layers:
                gather_moe(l + 1)
            if l < 2:
                phase_attnA(l, [])
                P.barrier()
                if stage == 2:
                    finish_stage()
                    return nc
                phase_wo(2, oA_loc, woA_in.ap()[l])
            else:
                phase_kv()
                P.barrier()
                phase_moba(l - 2, [])
                P.barrier()
                phase_wo(4, oB_loc, woB_in.ap()[l - 2])
            P.barrier()
            if stage == 3:
                finish_stage()
                return nc
            phase_tp(l, last=(l == n_layers - 1))
            P.barrier()
        P.emit()
    return nc


def _masksA():
    out = np.zeros((128, sum(NREL), 128), np.float32)
    k = np.arange(128)[:, None]
    q = np.arange(128)[None, :]
    off = 0
    for g, (win, d) in enumerate(A_GROUPS):
        for dt in range(NREL[g]):
            dist = dt * 128 + q - k
            out[:, off + dt, :] = (dist >= 0) & (dist % d == 0) & (dist // d <= win // d)
        off += NREL[g]
    return out.astype(NPBF)


def _abias(heads, nheads, ndt):
    out = np.zeros((128, len(heads), ndt), np.float32)
    k = np.arange(128, dtype=np.float64)[:, None]
    dt = np.arange(ndt, dtype=np.float64)[None, :]
    for i, h in enumerate(heads):
        slope = 2.0 ** (-8.0 * (h + 1) / nheads)
        out[:, i, :] = slope * (k - 64 - 128 * dt)
    return out


def prep_inputs(x, c, ada_down, ada_up, ada_bias, ln_gain, ln_bias, a_w_qkv, a_w_o, kv_ada_down, kv_ada_up,
                kv_ada_bias, kv_w, b_w_q, b_w_o, moe_w_group, moe_b_group, moe_w_expert, moe_b_expert,
                moe_w_gate, moe_w_up, moe_w_down):
    f = np.float32
    ca = np.ascontiguousarray
    xT = ca(np.asarray(x, f)[0].T)
    cT = ca(np.asarray(c, f)[0].reshape(32, 128).T)
    down_all = np.concatenate([np.asarray(ada_down, f), np.asarray(kv_ada_down, f)[None]], 0)
    lnT = np.zeros((128, 16, 32), f)
    for l in range(4):
        for i in range(2):
            lnT[:, (l * 2 + i) * 2 + 0, :] = np.asarray(ln_gain, f)[l, i].reshape(32, 128).T
            lnT[:, (l * 2 + i) * 2 + 1, :] = np.asarray(ln_bias, f)[l, i].reshape(32, 128).T
    wr = np.concatenate([np.asarray(moe_w_group, f),
                         np.asarray(moe_w_expert, f).transpose(0, 2, 1, 3).reshape(4, D, 32)], axis=2)
    rbv = np.concatenate([np.asarray(moe_b_group, f), np.asarray(moe_b_expert, f).reshape(4, 32)], axis=1)
    rb = ca(np.broadcast_to(rbv[None, :, None, :], (128, 4, 4, 36)))
    masks = _masksA()
    sel = np.zeros((32, 32, 128), f)
    for e in range(32):
        sel[e, e, :] = 1.0
    sel = sel.astype(NPBF)
    identf = np.eye(128, dtype=f)
    onesf = np.ones((128, 128), f)
    au = np.asarray(ada_up, f).reshape(4, 256, 6, D)
    ku = np.asarray(kv_ada_up, f).reshape(256, 2, D)
    ab = np.asarray(ada_bias, f).reshape(4, 6, D)
    kb = np.asarray(kv_ada_bias, f).reshape(2, D)
    wq6 = np.asarray(a_w_qkv, f).reshape(2, D, 3, 3, 16, 128)
    kvw_ = np.asarray(kv_w, f)
    in_maps = []
    for core in range(NCORE):
        fs = slice(512 * core, 512 * core + 512)
        upc = np.concatenate([au[:, :, :, fs].transpose(0, 2, 1, 3).reshape(24, 256, 512),
                              ku[:, :, fs].transpose(1, 0, 2)], 0)
        bvec = np.concatenate([ab[:, :, fs].reshape(24, 512), kb[:, fs]], 0)
        biasc = ca(bvec.reshape(26, 4, 128).transpose(2, 0, 1).reshape(128, 104))
        wqkvA = np.stack([np.stack([wq6[l, :, :, :, 2 * core + hh, :].transpose(0, 2, 1, 3).reshape(D, 1152)
                                    for hh in range(2)]) for l in range(2)])
        m = {
            "xT": ca(xT[:, TL * core:TL * (core + 1)]),
            "cT": cT,
            "downc": ca(down_all[:, :, 32 * core:32 * core + 32]),
            "upc": ca(upc),
            "biasc": biasc,
            "lnT": lnT,
            "wr": ca(wr),
            "rb": rb,
            "wqkvA": ca(wqkvA),
            "woA": ca(np.asarray(a_w_o, f)[:, 256 * core:256 * core + 256, :]),
            "masksA": masks,
            "abiasA": _abias([2 * core, 2 * core + 1], 16, 17),
            "wqB": ca(np.asarray(b_w_q, f)[:, :, 512 * core:512 * core + 512]),
            "kvw": ca(np.concatenate([kvw_[:, 128 * core:128 * core + 128],
                                      kvw_[:, 1024 + 128 * core:1024 + 128 * core + 128]], 1)),
            "woB": ca(np.asarray(b_w_o, f)[:, 512 * core:512 * core + 512, :]),
            "abiasB": _abias([4 * core + r for r in range(4)], 32, 64),
            "sel": sel,
            "wg": ca(np.asarray(moe_w_gate, f)[:, 4 * core:4 * core + 4].reshape(4, 4 * D, 256)),
            "wu": ca(np.asarray(moe_w_up, f)[:, 4 * core:4 * core + 4].reshape(4, 4 * D, 256)),
            "wd": ca(np.asarray(moe_w_down, f)[:, 4 * core:4 * core + 4].reshape(4, 4 * 256, D)),
            "identf": identf,
            "onesf": onesf,
        }
        in_maps.append(m)
    return in_maps


def kernel(**inputs):
    in_maps = prep_inputs(**inputs)
    nc = build(4)
    res = run_bass_kernel_spmd(nc, in_maps, core_ids=list(range(NCORE)))
    outT = np.concatenate([res.results[c]["outT"] for c in range(NCORE)], axis=1)
    return np.ascontiguousarray(outT.T)[None].astype(np.float32)
```
